# Optimizing a Trainium2 kernel written in Bass

```python
import jax, jax.numpy as jnp
from jax import lax
import numpy as np

D_MODEL = 4096
BATCH = 2
SEQ = 8192
DEPTH = 1

N_META = 16
MIX_WIDTH = D_MODEL
POOL_WINDOWS = (2, 4, 8, 16)
POOL_WIDTH = D_MODEL // 4
POOL_GROUP = POOL_WIDTH // len(POOL_WINDOWS)
QK_NOPE_DIM = 128
QK_ROPE_DIM = 64
QK_HEAD_DIM = QK_NOPE_DIM + QK_ROPE_DIM
V_HEAD_DIM = 128
MLA_WIDTH = MIX_WIDTH - POOL_WIDTH
MLA_HEADS = MLA_WIDTH // V_HEAD_DIM
Q_LORA_RANK = D_MODEL // 4
KV_LORA_RANK = 512
IN_COLS = POOL_WIDTH + Q_LORA_RANK + KV_LORA_RANK + QK_ROPE_DIM
ROPE_THETA = 10000.0
Q_BLOCK = 128
N_GROUPS = 8
EXPERTS_PER_GROUP = 8
N_EXPERTS = N_GROUPS * EXPERTS_PER_GROUP
TOP_K_INNER = 2
D_EXPERT = 512
MOE_BLOCK = 128
EPS = 1e-6

kernel_name = "hymba_pool_mla_hier_moe_block"


def rms_norm(x, g):
    xf = x.astype(jnp.float32)
    y = xf * lax.rsqrt(jnp.mean(xf * xf, axis=-1, keepdims=True) + EPS)
    return (y * g.astype(jnp.float32)).astype(x.dtype)


def rope_tables(length):
    inv = 1.0 / (ROPE_THETA ** (jnp.arange(0, QK_ROPE_DIM, 2, dtype=jnp.float32) / QK_ROPE_DIM))
    ang = jnp.arange(length, dtype=jnp.float32)[:, None] * inv[None, :]
    return jnp.cos(ang), jnp.sin(ang)


def apply_rope(x, cos, sin):
    xf = x.astype(jnp.float32)
    x1, x2 = jnp.split(xf, 2, axis=-1)
    c = cos[None, :, None, :]
    s = sin[None, :, None, :]
    return jnp.concatenate([x1 * c - x2 * s, x1 * s + x2 * c], axis=-1).astype(x.dtype)


def pool_mixer(u, w_pool, pool_scale):
    L = u.shape[1]
    count = jnp.arange(1, L + 1, dtype=jnp.float32)[None, :, None]
    outs = []
    for gi, w in enumerate(POOL_WINDOWS):
        ug = u[..., gi * POOL_GROUP:(gi + 1) * POOL_GROUP].astype(jnp.float32)
        cs = jnp.cumsum(ug, axis=1)
        lag = jnp.pad(cs, ((0, 0), (w, 0), (0, 0)))[:, :L]
        diff = ((cs - lag) / jnp.minimum(count, float(w)) - ug).astype(u.dtype)
        outs.append(diff @ w_pool[gi])
    return jnp.concatenate(outs, axis=-1) * pool_scale


def attend(qb, k, v, q_pos):
    s = jnp.einsum('bqhd,bkhd->bhqk', qb, k).astype(jnp.float32)
    key_pos = jnp.arange(k.shape[1])
    mask = key_pos[None, :] <= q_pos[:, None]
    s = jnp.where(mask[None, None], s, -jnp.inf)
    p = jax.nn.softmax(s, axis=-1).astype(v.dtype)
    return jnp.einsum('bhqk,bkhd->bqhd', p, v)


def mla_mixer(q_lat, kv_lat, k_rope, q_lat_norm_g, w_uq, kv_lat_norm_g, w_ukv, q_head_norm_g, k_head_norm_g, cos, sin):
    B, L, _ = q_lat.shape
    q = (rms_norm(q_lat, q_lat_norm_g) @ w_uq).reshape(B, L, MLA_HEADS, QK_HEAD_DIM)
    kv = (rms_norm(kv_lat, kv_lat_norm_g) @ w_ukv).reshape(B, L, MLA_HEADS, QK_NOPE_DIM + V_HEAD_DIM)
    k_nope = kv[..., :QK_NOPE_DIM]
    v = kv[..., QK_NOPE_DIM:]
    k_pe = jnp.broadcast_to(k_rope[:, :, None, :], (B, L, MLA_HEADS, QK_ROPE_DIM))
    k = jnp.concatenate([k_nope, k_pe], axis=-1)
    q = rms_norm(q, q_head_norm_g)
    k = rms_norm(k, k_head_norm_g)
    q = jnp.concatenate([q[..., :QK_NOPE_DIM], apply_rope(q[..., QK_NOPE_DIM:], cos, sin)], axis=-1)
    k = jnp.concatenate([k[..., :QK_NOPE_DIM], apply_rope(k[..., QK_NOPE_DIM:], cos, sin)], axis=-1)
    q = q * (QK_HEAD_DIM ** -0.5)
    o_meta = attend(q[:, :N_META], k[:, :N_META], v[:, :N_META], jnp.arange(N_META))
    n_real = L - N_META
    n_blk = n_real // Q_BLOCK
    q_real = q[:, N_META:].reshape(B, n_blk, Q_BLOCK, MLA_HEADS, QK_HEAD_DIM).transpose(1, 0, 2, 3, 4)
    pos = (N_META + jnp.arange(n_real)).reshape(n_blk, Q_BLOCK)
    o_real = lax.map(lambda a: attend(a[0], k, v, a[1]), (q_real, pos))
    o_real = o_real.transpose(1, 0, 2, 3, 4).reshape(B, n_real, MLA_HEADS, V_HEAD_DIM)
    o = jnp.concatenate([o_meta, o_real], axis=1)
    return o.reshape(B, L, MLA_WIDTH)


def hier_moe(h, w_group, b_group, w_expert, b_expert, w_gate, w_up, w_down):
    B, L, D = h.shape
    N = B * L
    xt = h.reshape(N, D)
    xf = xt.astype(jnp.float32)
    group_probs = jax.nn.softmax(xf @ w_group.astype(jnp.float32) + b_group.astype(jnp.float32), axis=-1)
    g_idx = jnp.argmax(group_probs, axis=-1)
    g_p = jnp.take_along_axis(group_probs, g_idx[:, None], axis=-1)
    e_logits = (xf @ w_expert.astype(jnp.float32) + b_expert.astype(jnp.float32)).reshape(N, N_GROUPS, EXPERTS_PER_GROUP)
    in_group = jnp.take_along_axis(e_logits, g_idx[:, None, None], axis=1)[:, 0]
    top_l, top_i = lax.top_k(in_group, TOP_K_INNER)
    gate = g_p * jax.nn.softmax(top_l, axis=-1)
    expert_id = g_idx[:, None] * EXPERTS_PER_GROUP + top_i
    A = N * TOP_K_INNER
    e_flat = expert_id.reshape(A).astype(jnp.int32)
    tok_flat = jnp.repeat(jnp.arange(N, dtype=jnp.int32), TOP_K_INNER)
    w_flat = gate.reshape(A)
    order = jnp.argsort(e_flat)
    e_s = e_flat[order]
    tok_s = tok_flat[order]
    w_s = w_flat[order]
    counts = jnp.bincount(e_flat, length=N_EXPERTS)
    padded = (counts + MOE_BLOCK - 1) // MOE_BLOCK * MOE_BLOCK
    start = jnp.cumsum(counts) - counts
    pend = jnp.cumsum(padded)
    pstart = pend - padded
    dest = pstart[e_s] + jnp.arange(A, dtype=jnp.int32) - start[e_s]
    n_blocks = (A + N_EXPERTS * (MOE_BLOCK - 1) + MOE_BLOCK - 1) // MOE_BLOCK
    P = n_blocks * MOE_BLOCK
    tok_buf = jnp.zeros((P,), jnp.int32).at[dest].set(tok_s)
    w_buf = jnp.zeros((P,), jnp.float32).at[dest].set(w_s)
    blk_start = jnp.arange(n_blocks, dtype=jnp.int32) * MOE_BLOCK
    blk_expert = jnp.minimum(jnp.searchsorted(pend, blk_start, side='right'), N_EXPERTS - 1)

    def run_block(args):
        tok, wt, e = args
        xb = xt[tok]
        hb = jax.nn.silu(xb @ w_gate[e]) * (xb @ w_up[e])
        return (hb @ w_down[e]) * wt[:, None].astype(xb.dtype)

    ys = lax.map(run_block, (tok_buf.reshape(n_blocks, MOE_BLOCK), w_buf.reshape(n_blocks, MOE_BLOCK), blk_expert))
    out = jax.ops.segment_sum(ys.reshape(P, D), tok_buf, num_segments=N)
    return out.reshape(B, L, D)


def setup_inputs(seed: int = 0) -> dict:
    key = jax.random.key(seed)
    ks = jax.random.split(key, 24)

    def nrm(k, shape, fan):
        return jax.random.normal(k, shape, jnp.float32) * (fan ** -0.5)

    def gain(k, shape):
        return 1.0 + 0.02 * jax.random.normal(k, shape, jnp.float32)

    return {
        "x": jax.random.normal(ks[0], (BATCH, SEQ, D_MODEL), jnp.float32),
        "meta_tokens": jax.random.normal(ks[1], (N_META, D_MODEL), jnp.float32),
        "mix_norm_g": gain(ks[2], (DEPTH, D_MODEL)),
        "w_in": nrm(ks[3], (DEPTH, D_MODEL, IN_COLS), D_MODEL),
        "q_lat_norm_g": gain(ks[4], (DEPTH, Q_LORA_RANK)),
        "w_uq": nrm(ks[5], (DEPTH, Q_LORA_RANK, MLA_HEADS * QK_HEAD_DIM), Q_LORA_RANK),
        "kv_lat_norm_g": gain(ks[6], (DEPTH, KV_LORA_RANK)),
        "w_ukv": nrm(ks[7], (DEPTH, KV_LORA_RANK, MLA_HEADS * (QK_NOPE_DIM + V_HEAD_DIM)), KV_LORA_RANK),
        "q_head_norm_g": gain(ks[8], (DEPTH, QK_HEAD_DIM)),
        "k_head_norm_g": gain(ks[9], (DEPTH, QK_HEAD_DIM)),
        "w_pool": nrm(ks[10], (DEPTH, len(POOL_WINDOWS), POOL_GROUP, POOL_GROUP), POOL_GROUP),
        "pool_scale": gain(ks[11], (DEPTH, POOL_WIDTH)),
        "w_out": nrm(ks[12], (DEPTH, MIX_WIDTH, D_MODEL), MIX_WIDTH),
        "ffn_norm_g": gain(ks[13], (DEPTH, D_MODEL)),
        "w_group": nrm(ks[14], (DEPTH, D_MODEL, N_GROUPS), D_MODEL),
        "b_group": 0.01 * jax.random.normal(ks[15], (DEPTH, N_GROUPS), jnp.float32),
        "w_expert": nrm(ks[16], (DEPTH, D_MODEL, N_EXPERTS), D_MODEL),
        "b_expert": 0.01 * jax.random.normal(ks[17], (DEPTH, N_EXPERTS), jnp.float32),
        "w_gate": nrm(ks[18], (DEPTH, N_EXPERTS, D_MODEL, D_EXPERT), D_MODEL),
        "w_up": nrm(ks[19], (DEPTH, N_EXPERTS, D_MODEL, D_EXPERT), D_MODEL),
        "w_down": nrm(ks[20], (DEPTH, N_EXPERTS, D_EXPERT, D_MODEL), D_EXPERT),
    }


def reference(x, meta_tokens, mix_norm_g, w_in, q_lat_norm_g, w_uq, kv_lat_norm_g, w_ukv, q_head_norm_g, k_head_norm_g, w_pool, pool_scale, w_out, ffn_norm_g, w_group, b_group, w_expert, b_expert, w_gate, w_up, w_down):
    B = x.shape[0]
    meta = jnp.broadcast_to(meta_tokens[None].astype(x.dtype), (B, N_META, D_MODEL))
    h = jnp.concatenate([meta, x], axis=1)
    L = h.shape[1]
    cos, sin = rope_tables(L)
    splits = [POOL_WIDTH, POOL_WIDTH + Q_LORA_RANK, POOL_WIDTH + Q_LORA_RANK + KV_LORA_RANK]
    for l in range(DEPTH):
        n = rms_norm(h, mix_norm_g[l])
        proj = n @ w_in[l]
        u, q_lat, kv_lat, k_rope = jnp.split(proj, splits, axis=-1)
        y_pool = pool_mixer(u, w_pool[l], pool_scale[l])
        y_mla = mla_mixer(q_lat, kv_lat, k_rope, q_lat_norm_g[l], w_uq[l], kv_lat_norm_g[l], w_ukv[l],
                          q_head_norm_g[l], k_head_norm_g[l], cos, sin)
        h = h + jnp.concatenate([y_pool, y_mla], axis=-1) @ w_out[l]
        h = h + hier_moe(rms_norm(h, ffn_norm_g[l]), w_group[l], b_group[l], w_expert[l], b_expert[l],
                         w_gate[l], w_up[l], w_down[l])
    return h[:, N_META:]
```

```python
import numpy as np
from contextlib import ExitStack
import concourse.bass as bass
import concourse.mybir as mybir
from concourse.bass_utils import run_bass_kernel_spmd

F32 = mybir.dt.float32
BF16 = mybir.dt.bfloat16
I32 = mybir.dt.int32
AF = mybir.ActivationFunctionType
ALU = mybir.AluOpType
AX = mybir.AxisListType

EPS = 1e-6
ENGS = ["pe", "act", "dve", "pool", "sp"]
NSEM_DMA = {"sp": 24, "pool": 24, "act": 6}


class Cfg:
    def __init__(self, D=4096, KL=512, DE=512, NE=64, NSLOT=4):
        SEQ = 2048 * NSLOT
        self.D = D
        self.KL = KL
        self.DE = DE
        self.NE = NE
        self.NG = 8
        self.EPG = NE // 8
        self.SEQ = SEQ
        self.NMETA = 16
        self.R = 64
        self.PW = D // 4
        self.PG = self.PW // 4
        self.QL = D // 4
        self.H = (D - self.PW) // 128
        self.DC = D // 128
        self.KC = KL // 128
        self.QC = self.QL // 128
        self.PC = self.PW // 128
        self.PGC = self.PG // 128
        self.EC = DE // 128
        self.TS = 512
        self.NSLOT = NSLOT
        self.NT = SEQ // 512
        self.NKB = SEQ // 128 + 1
        self.LKP = self.NKB * 128
        self.NOWN = 512 * NSLOT
        self.NRC = 8 + NE
        self.CT = D // 512


class Prog:
    def __init__(self):
        self.ops = []
        self.res = {}
        self.last = {e: None for e in ENGS}
        self.dma_n = {q: 0 for q in NSEM_DMA}
        self.dma_last = {}

    def _add(self, eng, kind, fn, reads, writes, extra=(), semkey=None):
        deps = set(extra)
        for r in reads:
            e = self.res.get(r)
            if e and e[0] is not None:
                deps.add(e[0])
        for w in writes:
            e = self.res.get(w)
            if e:
                if e[0] is not None:
                    deps.add(e[0])
                deps.update(e[1])
        oid = len(self.ops)
        self.ops.append(dict(eng=eng, kind=kind, fn=fn, deps=deps, sig=False, semkey=semkey, val=None))
        for r in reads:
            self.res.setdefault(r, [None, []])[1].append(oid)
        for w in writes:
            self.res[w] = [oid, []]
        self.last[eng] = oid
        return oid

    def op(self, eng, fn, reads=(), writes=()):
        return self._add(eng, "c", fn, reads, writes)

    def dma(self, q, fn, reads=(), writes=()):
        i = self.dma_n[q] % NSEM_DMA[q]
        self.dma_n[q] += 1
        key = (q, i)
        extra = ()
        if key in self.dma_last:
            extra = (self.dma_last[key],)
        oid = self._add(q, "d", fn, reads, writes, extra=extra, semkey=key)
        self.dma_last[key] = oid
        return oid

    def barrier(self):
        import os as _o
        if 'KVERB' in _o.environ:
            print('barrier at op', len(self.ops))
        deps = set()
        for e in ENGS:
            if self.last[e] is not None:
                deps.add(self.last[e])
        deps.update(self.dma_last.values())
        for e in ENGS:
            oid = len(self.ops)
            self.ops.append(dict(eng=e, kind="b", fn=None, deps=set(deps), sig=False, semkey=None, val=None))
            self.last[e] = oid
        self.res = {}

    def emit(self, nc, es):
        ops = self.ops
        for o in ops:
            for d in o["deps"]:
                od = ops[d]
                if od["kind"] == "c":
                    od["sig"] = True
        cnt = {e: 0 for e in ENGS}
        dcnt = {}
        for o in ops:
            if o["kind"] == "c" and o["sig"]:
                cnt[o["eng"]] += 1
                o["val"] = cnt[o["eng"]]
            elif o["kind"] == "d":
                dcnt[o["semkey"]] = dcnt.get(o["semkey"], 0) + 16
                o["val"] = dcnt[o["semkey"]]
        esem = {e: es.enter_context(nc.semaphore("se_" + e)) for e in ENGS}
        dsem = {}
        for q, n in NSEM_DMA.items():
            for i in range(n):
                dsem[(q, i)] = es.enter_context(nc.semaphore("sd_%s_%d" % (q, i)))
        per = {e: [] for e in ENGS}
        for o in ops:
            per[o["eng"]].append(o)

        def run(eng_name, e):
            waited = {}
            for o in per[eng_name]:
                need = {}
                for d in o["deps"]:
                    od = ops[d]
                    if od["kind"] == "c":
                        k = ("e", od["eng"])
                        sem = esem[od["eng"]]
                    elif od["kind"] == "d":
                        k = ("d", od["semkey"])
                        sem = dsem[od["semkey"]]
                    else:
                        continue
                    v = od["val"]
                    if waited.get(k, 0) >= v:
                        continue
                    if k not in need or need[k][1] < v:
                        need[k] = (sem, v)
                for k, (sem, v) in need.items():
                    e.wait_ge(sem, v)
                    waited[k] = v
                if o["fn"] is None:
                    continue
                ins = o["fn"](e)
                if o["kind"] == "d":
                    ins.then_inc(dsem[o["semkey"]], 16)
                elif o["sig"]:
                    ins.then_inc(esem[eng_name], 1)

        with nc.Block() as block:
            @block.tensor
            def _(e):
                run("pe", e)

            @block.scalar
            def _(e):
                run("act", e)

            @block.vector
            def _(e):
                run("dve", e)

            @block.gpsimd
            def _(e):
                run("pool", e)

            @block.sync
            def _(e):
                run("sp", e)


class Ring:
    def __init__(self, name, aps):
        self.name = name
        self.aps = aps
        self.i = -1

    def next(self):
        self.i = (self.i + 1) % len(self.aps)
        return "%s%d" % (self.name, self.i), self.aps[self.i]


def build(cfg, STOP=None):
    c = cfg
    nc = bass.Bass("TRN2", target_bir_lowering=False)
    es = ExitStack()
    P = Prog()
    D, H, DC, KC, QC, PC, KL, QL, PW, DE, NE = c.D, c.H, c.DC, c.KC, c.QC, c.PC, c.KL, c.QL, c.PW, c.DE, c.NE
    LKP, NKB = c.LKP, c.NKB

    def din(name, shape, dt=F32):
        return nc.dram_tensor(name, list(shape), dt, kind="ExternalInput")

    def dscr(name, shape, dt):
        return nc.dram_tensor(name, list(shape), dt)

    xb = din("xb", [c.SEQ, D])
    meta = din("meta", [c.NMETA, D])
    xo = din("xo", [c.NSLOT * 528, D])
    cosk = din("cosk", [64, LKP])
    sink = din("sink", [64, LKP])
    cosq = din("cosq", [64, c.NOWN])
    sinq = din("sinq", [64, c.NOWN])
    maskc = din("maskc", [128, 16, 512])
    ident_d = din("ident", [128, 128])
    ones_d = din("ones", [128, 128])
    tri_d = din("tri", [128, 128])
    iota_d = din("iotae", [128, NE])
    gmix_bc = din("gmix_bc", [128, D])
    gffn_bc = din("gffn_bc", [128, D])
    w_kv = din("w_kv", [D, KL + 128])
    w_own = din("w_own", [D, PW + QL])
    gkv = din("gkv", [128, KC])
    gql = din("gql", [128, QC])
    wk = din("wk", [KL, H * 128])
    wv = din("wv", [KL, H * 128])
    wqn = din("wqn", [QL, H * 128])
    wqr = din("wqr", [QL, H * 64])
    wqs = din("wqs", [QL, H * 64])
    gk = din("gk", [128, 3])
    gq = din("gq", [128, 3])
    wpool = din("wpool", [4, c.PG, c.PG])
    pscale = din("pscale", [128, PC])
    w_out = din("w_out", [D, D])
    w_r = din("w_r", [D, c.NRC])
    b_r = din("b_r", [128, c.NRC])
    w_gate = din("w_gate", [NE, D, DE])
    w_up = din("w_up", [NE, D, DE])
    w_down = din("w_down", [NE, DE, D])
    out = nc.dram_tensor("out", [c.NOWN, D], F32, kind="ExternalOutput")

    KTn = dscr("KTn", [H, 128, LKP], BF16)
    KTr = dscr("KTr", [H, 64, LKP], BF16)
    Vd = dscr("Vd", [H, 128, NKB, 128], BF16)
    QTn = dscr("QTn", [H, 128, c.NOWN], BF16)
    QTr = dscr("QTr", [H, 64, c.NOWN], BF16)
    qnT_d = dscr("qnT_d", [QC, 128, c.NOWN], BF16)
    mixT = dscr("mixT", [DC, 128, c.NOWN], BF16)
    kvn_d = dscr("kvn_d", [KC, 128, LKP], BF16)
    kpsq_d = dscr("kpsq_d", [64, LKP], BF16)
    RT_d = dscr("RT_d", [64, LKP], F32)
    Xe = dscr("Xe", [NE * 128, D], BF16)
    Ye = dscr("Ye", [NE * 128, D], F32)

    state = {"off": 16512, "n": 0}

    def sb(shape, dt, name="t"):
        per = 1
        for s_ in shape[1:]:
            per *= s_
        nbytes = per * (4 if dt in (F32, I32) else 2)
        nbytes = (nbytes + 63) // 64 * 64
        off = state["off"]
        state["off"] += nbytes
        assert state["off"] <= 229376, ("sbuf overflow", name, state["off"])
        state["n"] += 1
        return nc.alloc_sbuf_tensor_at("%s_%d" % (name, state["n"]), list(shape), dt, offset=off)

    ident = sb([128, 128], BF16, "ident")
    ones = sb([128, 128], BF16, "ones")
    small = sb([128, 64], F32, "small")
    gk_s = sb([128, 3], F32, "gk")
    gq_s = sb([128, 3], F32, "gq")
    dest_all = sb([128, 16, 2], I32, "dest")
    wgt_all = sb([128, 16, 2], F32, "wgt")
    PERSIST = state["off"]

    ps = [es.enter_context(nc.psum_tensor("ps%d" % i, [128, 512], F32)) for i in range(8)]

    def psbf(i):
        return ps[i][:, :].bitcast(BF16)

    def ld_cast(dst_ap, src_ap, name, q="pool"):
        return P.dma("pool", lambda e: e.dma_start(out=dst_ap, in_=src_ap), writes=[name])

    def ld(dst_ap, src_ap, name, q="sp"):
        return P.dma(q, lambda e: e.dma_start(out=dst_ap, in_=src_ap), writes=[name])

    def st(dst_ap, src_ap, name, q="sp"):
        return P.dma(q, lambda e: e.dma_start(out=dst_ap, in_=src_ap), reads=[name])

    ld_cast(ident[:, :], ident_d[:, :], "ident")
    ld_cast(ones[:, :], ones_d[:, :], "ones")
    ld(gk_s[:, :], gk[:, :], "gk")
    ld(gq_s[:, :], gq[:, :], "gq")
    P.op("dve", lambda e: e.tensor_scalar(out=gq_s[:, :], in0=gq_s[:, :], scalar1=float(192.0 ** -0.5),
                                          scalar2=None, op0=ALU.mult), reads=["gq"], writes=["gq"])
    P.barrier()

    def rms_rows(xt_name, xt, rows, junk_name, junk, g_bc, xn_name, xn, ssq_ap, scr_name):
        P.op("act", lambda e: e.activation(out=junk[:rows, :], in_=xt[:rows, :], func=AF.Square,
                                           accum_out=ssq_ap[:rows, 0:1]),
             reads=[xt_name], writes=[junk_name, scr_name])
        P.op("act", lambda e: e.activation(out=ssq_ap[:rows, 1:2], in_=ssq_ap[:rows, 0:1], func=AF.Ln,
                                           scale=1.0 / D, bias=eps_t[:rows, 0:1]),
             reads=[scr_name, "eps"], writes=[scr_name])
        P.op("act", lambda e: e.activation(out=ssq_ap[:rows, 2:3], in_=ssq_ap[:rows, 1:2], func=AF.Exp,
                                           scale=-0.5),
             reads=[scr_name], writes=[scr_name])
        P.op("dve", lambda e: e.scalar_tensor_tensor(out=xn[:rows, :], in0=xt[:rows, :], scalar=ssq_ap[:rows, 2:3],
                                                     in1=g_bc[:rows, :], op0=ALU.mult, op1=ALU.mult),
             reads=[xt_name, scr_name, "gbc"], writes=[xn_name, junk_name])

    eps_t = sb([128, 1], F32, "eps")
    P.op("dve", lambda e: e.memset(eps_t[:, :], EPS), writes=["eps"])
    PERSIST = state["off"]

    tr_state = {"i": 0}

    def transpose_block(src_name, src, rows, nch, dst_name, dst, col0, banks=(0, 1)):
        for g0 in range(0, nch, 8):
            ng = min(8, nch - g0)
            bi = banks[tr_state["i"] % len(banks)]
            tr_state["i"] += 1
            pv = psbf(bi)

            def f(e, g0=g0, ng=ng, pv=pv):
                ins = None
                for k in range(ng):
                    ins = e.transpose(out=pv[:, k * 128:k * 128 + rows], in_=src[:rows, (g0 + k) * 128:(g0 + k + 1) * 128],
                                      identity=ident[:rows, :rows])
                return ins
            P.op("pe", f, reads=[src_name, "ident"], writes=["ps%d" % bi])
            eng = "act" if (tr_state["i"] % 2) else "dve"
            pv3 = pv[:, 0:ng * 128].rearrange("p (k r) -> p k r", r=128)

            def g(e, g0=g0, ng=ng, pv3=pv3, eng=eng):
                if eng == "act":
                    return e.activation(out=dst[:, g0:g0 + ng, col0:col0 + rows], in_=pv3[:, :, 0:rows], func=AF.Copy)
                return e.tensor_copy(out=dst[:, g0:g0 + ng, col0:col0 + rows], in_=pv3[:, :, 0:rows])
            P.op(eng, g, reads=["ps%d" % bi], writes=[dst_name])

    def rstd_from_psum(ssq_bank, n_feat, nt, lnv_name, lnv, rstd_name, rstd):
        P.op("act", lambda e: e.activation(out=lnv[:, :nt], in_=ps[ssq_bank][:, :nt], func=AF.Ln,
                                           scale=1.0 / n_feat, bias=eps_t[:, 0:1]),
             reads=["ps%d" % ssq_bank, "eps"], writes=[lnv_name])
        P.op("act", lambda e: e.activation(out=rstd[:, :nt], in_=lnv[:, :nt], func=AF.Exp, scale=-0.5),
             reads=[lnv_name], writes=[rstd_name])

    def phase_0():
        state["off"] = PERSIST
        Wkv = sb([128, DC, KL + 128], BF16, "Wkv")
        gbc = sb([128, D], F32, "gbc")
        gkv_s = sb([128, KC], F32, "gkv")
        xt_r = Ring("xt", [sb([128, D], F32, "xt") for _ in range(2)])
        xnb = sb([128, D], BF16, "xnb")
        xnT = sb([128, DC, 512], BF16, "xnT")
        stat_r = Ring("stat", [sb([128, 4], F32, "stat") for _ in range(2)])
        kvraw = sb([128, KC, 512], F32, "kvraw")
        sqb_r = Ring("sqb", [sb([128, 512], BF16, "sqb") for _ in range(2)])
        kvnT_r = Ring("kvnT", [sb([128, KC, 512], BF16, "kvnT") for _ in range(2)])
        lnv = sb([128, 512], F32, "lnv")
        rstd_r = Ring("rstd", [sb([128, 512], F32, "rstd") for _ in range(2)])
        cs_t = sb([64, 2, 512], F32, "cs")
        kpsq_r = Ring("kpsq", [sb([64, 512], BF16, "kpsq") for _ in range(2)])
        rt_t = sb([64, 2, 512], F32, "rt")
        RT_r = Ring("RT", [sb([64, 512], F32, "RT") for _ in range(2)])

        ld_cast(Wkv[:, :, :], w_kv.ap().rearrange("(c p) n -> p c n", p=128), "Wkv")
        ld(gbc[:, :], gmix_bc[:, :], "gbc")
        ld(gkv_s[:, :], gkv[:, :], "gkv")

        for ti in range(c.NT + 1):
            ntk = 512 if ti < c.NT else c.NMETA
            k0 = ti * 512
            nblk = (ntk + 127) // 128
            ld(cs_t[:, 0, :ntk], cosk[:, k0:k0 + ntk], "cs")
            ld(cs_t[:, 1, :ntk], sink[:, k0:k0 + ntk], "cs")
            for blk in range(nblk):
                rows = min(128, ntk - blk * 128)
                xtn, xt = xt_r.next()
                src = xb[k0 + blk * 128:k0 + blk * 128 + rows, :] if ti < c.NT else meta[0:rows, :]
                P.dma("sp", lambda e, xt=xt, src=src, rows=rows: e.dma_start(out=xt[:rows, :], in_=src), writes=[xtn])
                stn, stt_ = stat_r.next()
                rms_rows(xtn, xt, rows, "xnb", xnb, gbc, "xnb", xnb, stt_, stn)
                transpose_block("xnb", xnb, rows, DC, "xnT", xnT, blk * 128)
            for m in range(KC):
                bank = 2 + (m % 2)

                def f(e, m=m, bank=bank, ntk=ntk):
                    ins = None
                    for k in range(DC):
                        ins = e.matmul(ps[bank][:, :ntk], lhsT=Wkv[:, k, m * 128:(m + 1) * 128], rhs=xnT[:, k, :ntk],
                                       start=(k == 0), stop=(k == DC - 1))
                    return ins
                P.op("pe", f, reads=["xnT", "Wkv"], writes=["ps%d" % bank])
                P.op("act", lambda e, m=m, bank=bank, ntk=ntk: e.activation(out=kvraw[:, m, :ntk], in_=ps[bank][:, :ntk], func=AF.Copy),
                     reads=["ps%d" % bank], writes=["kvraw%d" % m])
                sqn, sq = sqb_r.next()
                P.op("dve", lambda e, m=m, sq=sq, ntk=ntk: e.tensor_tensor(out=sq[:, :ntk], in0=kvraw[:, m, :ntk], in1=kvraw[:, m, :ntk], op=ALU.mult),
                     reads=["kvraw%d" % m], writes=[sqn])
                P.op("pe", lambda e, m=m, sq=sq, ntk=ntk: e.matmul(ps[4][:, :ntk], lhsT=ones[:, :], rhs=sq[:, :ntk], start=(m == 0), stop=(m == KC - 1)),
                     reads=[sqn, "ones"], writes=["ps4"])
            rsn, rs = rstd_r.next()
            rstd_from_psum(4, KL, ntk, "lnv", lnv, rsn, rs)
            kvn, kvnT = kvnT_r.next()
            for m in range(KC):
                P.op("dve", lambda e, m=m, rs=rs, kvnT=kvnT, ntk=ntk: e.scalar_tensor_tensor(
                    out=kvnT[:, m, :ntk], in0=kvraw[:, m, :ntk], scalar=gkv_s[:, m:m + 1], in1=rs[:, :ntk], op0=ALU.mult, op1=ALU.mult),
                    reads=["kvraw%d" % m, rsn, "gkv"], writes=[kvn])
            st(kvn_d[:, :, k0:k0 + ntk].rearrange("c p n -> p c n"), kvnT[:, :, :ntk], kvn)
            for v in range(2):
                bank = 5 + v

                def f(e, v=v, bank=bank, ntk=ntk):
                    ins = None
                    for k in range(DC):
                        ins = e.matmul(ps[bank][:64, :ntk], lhsT=Wkv[:, k, KL + 64 * v:KL + 64 * v + 64], rhs=xnT[:, k, :ntk],
                                       start=(k == 0), stop=(k == DC - 1))
                    return ins
                P.op("pe", f, reads=["xnT", "Wkv"], writes=["ps%d" % bank])
            kpn, kpsq = kpsq_r.next()
            P.op("act", lambda e, ntk=ntk, kpsq=kpsq: e.activation(out=kpsq[:, :ntk], in_=ps[5][:64, :ntk], func=AF.Square),
                 reads=["ps5"], writes=[kpn])
            st(kpsq_d[:, k0:k0 + ntk], kpsq[:, :ntk], kpn)
            for v in range(2):
                P.op("act", lambda e, v=v, ntk=ntk: e.activation(out=rt_t[:, v, :ntk], in_=ps[5 + v][:64, :ntk], func=AF.Copy,
                                                                 scale=gk_s[:64, 1 + v:2 + v]),
                     reads=["ps%d" % (5 + v), "gk"], writes=["rt%d" % v])
                P.op("dve", lambda e, v=v, ntk=ntk: e.tensor_tensor(out=rt_t[:, v, :ntk], in0=rt_t[:, v, :ntk], in1=cs_t[:, v, :ntk], op=ALU.mult),
                     reads=["rt%d" % v, "cs"], writes=["rt%d" % v])
            RTn, RT = RT_r.next()
            P.op("dve", lambda e, ntk=ntk, RT=RT: e.tensor_tensor(out=RT[:, :ntk], in0=rt_t[:, 0, :ntk], in1=rt_t[:, 1, :ntk], op=ALU.add),
                 reads=["rt0", "rt1"], writes=[RTn])
            st(RT_d[:, k0:k0 + ntk], RT[:, :ntk], RTn)
        P.barrier()

        state["off"] = PERSIST
        wk_s = sb([128, KC, H * 128], BF16, "wk")
        wv_s = sb([128, KC, H * 128], BF16, "wv")
        kvnT_r = Ring("kvnT", [sb([128, KC, 512], BF16, "kvnT") for _ in range(2)])
        kpsq_r = Ring("kpsq", [sb([64, 512], BF16, "kpsq") for _ in range(2)])
        RT_r = Ring("RT", [sb([64, 512], F32, "RT") for _ in range(2)])
        sqb_r = Ring("sqb", [sb([128, 512], BF16, "sqb") for _ in range(2)])
        lnv = sb([128, 512], F32, "lnv")
        rstd_r = Ring("rstd", [sb([128, 512], F32, "rstd") for _ in range(2)])
        kn_r = Ring("kn", [sb([128, 512], BF16, "kn") for _ in range(3)])
        kr_r = Ring("kr", [sb([64, 512], BF16, "kr") for _ in range(3)])
        vs_r = Ring("vs", [sb([128, 4, 128], BF16, "vs") for _ in range(4)])
        ktmp_r = Ring("ktmp", [sb([128, 512], F32, "ktmp") for _ in range(2)])
        ld_cast(wk_s[:, :, :], wk.ap().rearrange("(c p) n -> p c n", p=128), "wk")
        ld_cast(wv_s[:, :, :], wv.ap().rearrange("(c p) n -> p c n", p=128), "wv")
        for ti in range(c.NT + 1):
            ntk = 512 if ti < c.NT else c.NMETA
            k0 = ti * 512
            nblk = (ntk + 127) // 128
            kvn, kvnT = kvnT_r.next()
            kpn, kpsq = kpsq_r.next()
            RTn, RT = RT_r.next()
            ld(kvnT[:, :, :ntk], kvn_d[:, :, k0:k0 + ntk].rearrange("c p n -> p c n"), kvn)
            ld(kpsq[:, :ntk], kpsq_d[:, k0:k0 + ntk], kpn)
            ld(RT[:, :ntk], RT_d[:, k0:k0 + ntk], RTn)
            for h in range(H):
                bank = 2 + (h % 2)

                def f(e, h=h, bank=bank, kvnT=kvnT, ntk=ntk):
                    ins = None
                    for m in range(KC):
                        ins = e.matmul(ps[bank][:, :ntk], lhsT=wk_s[:, m, h * 128:(h + 1) * 128], rhs=kvnT[:, m, :ntk],
                                       start=(m == 0), stop=(m == KC - 1))
                    return ins
                P.op("pe", f, reads=[kvn, "wk"], writes=["ps%d" % bank])
                sqn, sq = sqb_r.next()
                P.op("act", lambda e, sq=sq, bank=bank, ntk=ntk: e.activation(out=sq[:, :ntk], in_=ps[bank][:, :ntk], func=AF.Square),
                     reads=["ps%d" % bank], writes=[sqn])

                def f2(e, sq=sq, ntk=ntk, kpsq=kpsq):
                    e.matmul(ps[4][:, :ntk], lhsT=ones[:, :], rhs=sq[:, :ntk], start=True, stop=False)
                    return e.matmul(ps[4][:, :ntk], lhsT=ones[:64, :], rhs=kpsq[:, :ntk], start=False, stop=True)
                P.op("pe", f2, reads=[sqn, kpn, "ones"], writes=["ps4"])
                rsn, rs = rstd_r.next()
                rstd_from_psum(4, 192, ntk, "lnv", lnv, rsn, rs)
                knn, kn = kn_r.next()
                ktn_, ktmp = ktmp_r.next()
                P.op("act", lambda e, ktmp=ktmp, bank=bank, ntk=ntk: e.activation(out=ktmp[:, :ntk], in_=ps[bank][:, :ntk], func=AF.Copy,
                                                                                 scale=gk_s[:, 0:1]),
                     reads=["ps%d" % bank, "gk"], writes=[ktn_])
                P.op("dve", lambda e, kn=kn, rs=rs, ktmp=ktmp, ntk=ntk: e.tensor_tensor(out=kn[:, :ntk], in0=ktmp[:, :ntk], in1=rs[:, :ntk], op=ALU.mult),
                     reads=[ktn_, rsn], writes=[knn])
                krn, kr = kr_r.next()
                P.op("dve", lambda e, kr=kr, rs=rs, ntk=ntk, RT=RT: e.tensor_tensor(out=kr[:, :ntk], in0=RT[:, :ntk], in1=rs[:64, :ntk], op=ALU.mult),
                     reads=[RTn, rsn], writes=[krn])
                st(KTn[h, :, k0:k0 + ntk], kn[:, :ntk], knn)
                st(KTr[h, :, k0:k0 + ntk], kr[:, :ntk], krn)
            for blk in range(nblk):
                rows = min(128, ntk - blk * 128)
                for hg in range(H // 4):
                    bank = 6 + (hg % 2)

                    def f(e, blk=blk, rows=rows, hg=hg, bank=bank, kvnT=kvnT):
                        ins = None
                        for m in range(KC):
                            ins = e.matmul(ps[bank][:rows, :], lhsT=kvnT[:, m, blk * 128:blk * 128 + rows], rhs=wv_s[:, m, hg * 512:(hg + 1) * 512],
                                           start=(m == 0), stop=(m == KC - 1))
                        return ins
                    P.op("pe", f, reads=[kvn, "wv"], writes=["ps%d" % bank])
                    vsn, vs = vs_r.next()
                    psv = ps[bank][:rows, :].rearrange("p (h d) -> p h d", d=128)
                    if hg % 2 == 0:
                        P.op("act", lambda e, rows=rows, vs=vs, psv=psv: e.activation(out=vs[:rows, :, :], in_=psv, func=AF.Copy),
                             reads=["ps%d" % bank], writes=[vsn])
                    else:
                        P.op("dve", lambda e, rows=rows, vs=vs, psv=psv: e.tensor_copy(out=vs[:rows, :, :], in_=psv),
                             reads=["ps%d" % bank], writes=[vsn])
                    st(Vd[hg * 4:(hg + 1) * 4, :rows, ti * 4 + blk, :].rearrange("h p d -> p h d"), vs[:rows, :, :], vsn)
        P.barrier()


    if STOP is None or 0 <= STOP:
        phase_0()

    def phase_1():
        nonlocal_dummy = None
        state["off"] = PERSIST
        gbc = sb([128, D], F32, "gbc")
        xt_r = Ring("xt", [sb([128, D], F32, "xt") for _ in range(2)])
        xnb = sb([128, D], BF16, "xnb")
        xnT = sb([128, DC, 528], BF16, "xnT")
        stat_r = Ring("stat", [sb([128, 4], F32, "stat") for _ in range(2)])
        wt_r = Ring("wt", [sb([128, DC, 128], BF16, "wt") for _ in range(3)])
        Upad = sb([128, PC, 528], F32, "Upad")
        sA = sb([128, c.PGC, 528], F32, "sA")
        sB = sb([128, c.PGC, 528], F32, "sB")
        diffT = sb([128, PC, 512], BF16, "diffT")
        wp_s = sb([128, 4 * c.PGC, c.PG], BF16, "wp")
        psc_s = sb([128, PC], F32, "psc")
        gql_s = sb([128, QC], F32, "gql")
        qraw = sb([128, QC, 512], F32, "qraw")
        sqb_r = Ring("sqb", [sb([128, 512], BF16, "sqb") for _ in range(2)])
        lnv = sb([128, 512], F32, "lnv")
        rstd = sb([128, 512], F32, "rstd")
        yp_r = Ring("yp", [sb([128, 512], BF16, "yp") for _ in range(3)])

        ld(gbc[:, :], gmix_bc[:, :], "gbc")
        ld_cast(wp_s[:, :, :], wpool.ap().rearrange("g (c p) n -> p (g c) n", p=128), "wp")
        ld(psc_s[:, :], pscale[:, :], "psc")
        ld(gql_s[:, :], gql[:, :], "gql")
        w_own_v = w_own.ap().rearrange("(c p) n -> p c n", p=128)

        for s in range(c.NSLOT):
            for blk in range(5):
                rows = 128 if blk < 4 else 16
                xtn, xt = xt_r.next()
                r0 = s * 528 + blk * 128
                P.dma("sp", lambda e, xt=xt, r0=r0, rows=rows: e.dma_start(out=xt[:rows, :], in_=xo[r0:r0 + rows, :]), writes=[xtn])
                stn, stt_ = stat_r.next()
                rms_rows(xtn, xt, rows, "xnb", xnb, gbc, "xnb", xnb, stt_, stn)
                transpose_block("xnb", xnb, rows, DC, "xnT", xnT, blk * 128)
            for ct in range(PC + QC):
                wtn, wt = wt_r.next()
                ld_cast(wt[:, :, :], w_own_v[:, :, ct * 128:(ct + 1) * 128], wtn)
                bank = 2 + (ct % 2)

                def f(e, wt=wt, bank=bank):
                    ins = None
                    for k in range(DC):
                        ins = e.matmul(ps[bank][:, :], lhsT=wt[:, k, :], rhs=xnT[:, k, 0:512], start=(k == 0), stop=(k == DC - 1))
                    return ins
                P.op("pe", f, reads=["xnT", wtn], writes=["ps%d" % bank])
                if ct < PC:
                    def f2(e, wt=wt):
                        ins = None
                        for k in range(DC):
                            ins = e.matmul(ps[4][:, 0:16], lhsT=wt[:, k, :], rhs=xnT[:, k, 512:528], start=(k == 0), stop=(k == DC - 1))
                        return ins
                    P.op("pe", f2, reads=["xnT", wtn], writes=["ps4"])
                    P.op("act", lambda e, ct=ct, bank=bank: e.activation(out=Upad[:, ct, 16:528], in_=ps[bank][:, :], func=AF.Copy),
                         reads=["ps%d" % bank], writes=["Upad%d" % ct])
                    P.op("dve", lambda e, ct=ct: e.tensor_copy(out=Upad[:, ct, 0:16], in_=ps[4][:, 0:16]),
                         reads=["ps4"], writes=["Upad%d" % ct])
                else:
                    m = ct - PC
                    P.op("act", lambda e, m=m, bank=bank: e.activation(out=qraw[:, m, :], in_=ps[bank][:, :], func=AF.Copy),
                         reads=["ps%d" % bank], writes=["qraw%d" % m])
                    sqn, sq = sqb_r.next()
                    P.op("dve", lambda e, m=m, sq=sq: e.tensor_tensor(out=sq[:, :], in0=qraw[:, m, :], in1=qraw[:, m, :], op=ALU.mult),
                         reads=["qraw%d" % m], writes=[sqn])
                    P.op("pe", lambda e, m=m, sq=sq: e.matmul(ps[5][:, :], lhsT=ones[:, :], rhs=sq[:, :], start=(m == 0), stop=(m == QC - 1)),
                         reads=[sqn, "ones"], writes=["ps5"])
            rstd_from_psum(5, QL, 512, "lnv", lnv, "rstd", rstd)
            for m in range(QC):
                ypn, yp = yp_r.next()
                P.op("dve", lambda e, m=m, yp=yp: e.scalar_tensor_tensor(out=yp[:, :], in0=qraw[:, m, :], scalar=gql_s[:, m:m + 1],
                                                                          in1=rstd[:, :], op0=ALU.mult, op1=ALU.mult),
                     reads=["qraw%d" % m, "rstd", "gql"], writes=[ypn])
                st(qnT_d[m, :, s * 512:(s + 1) * 512], yp[:, :], ypn)
            for gi in range(4):
                w = 2 << gi
                c0 = gi * c.PGC
                ures = ["Upad%d" % cc for cc in range(c0, c0 + c.PGC)]
                U = Upad[:, c0:c0 + c.PGC, :]
                cur, cur_name = U, None
                sh = 1
                bufs = [(sA, "sA"), (sB, "sB")]
                bi = 0
                while sh < w:
                    dstb, dstn = bufs[bi]
                    bi ^= 1
                    P.op("dve", lambda e, cur=cur, dstb=dstb, sh=sh: e.tensor_tensor(
                        out=dstb[:, :, 2 * sh - 1:528], in0=cur[:, :, 2 * sh - 1:528], in1=cur[:, :, sh - 1:528 - sh], op=ALU.add),
                        reads=(ures if cur_name is None else [cur_name]), writes=[dstn])
                    cur, cur_name = dstb, dstn
                    sh *= 2
                P.op("dve", lambda e, cur=cur, U=U, c0=c0, w=w: e.scalar_tensor_tensor(
                    out=diffT[:, c0:c0 + c.PGC, :], in0=cur[:, :, 16:528], scalar=1.0 / w, in1=U[:, :, 16:528],
                    op0=ALU.mult, op1=ALU.subtract), reads=ures + [cur_name], writes=["diffT%d" % gi])
                for co in range(c.PGC):
                    bank = 6 + (co % 2)

                    def f(e, gi=gi, co=co, bank=bank, c0=c0):
                        ins = None
                        for ci in range(c.PGC):
                            ins = e.matmul(ps[bank][:, :], lhsT=wp_s[:, gi * c.PGC + ci, co * 128:(co + 1) * 128], rhs=diffT[:, c0 + ci, :],
                                           start=(ci == 0), stop=(ci == c.PGC - 1))
                        return ins
                    P.op("pe", f, reads=["diffT%d" % gi, "wp"], writes=["ps%d" % bank])
                    ypn, yp = yp_r.next()
                    ch = c0 + co
                    P.op("act", lambda e, yp=yp, bank=bank, ch=ch: e.activation(out=yp[:, :], in_=ps[bank][:, :], func=AF.Copy,
                                                                                scale=psc_s[:, ch:ch + 1]),
                         reads=["ps%d" % bank, "psc"], writes=[ypn])
                    st(mixT[ch, :, s * 512:(s + 1) * 512], yp[:, :], ypn)
        P.barrier()

    if STOP is None or 1 <= STOP:
        phase_1()

    def phase_2():
        nonlocal_dummy = None
        state["off"] = PERSIST
        qnT = sb([128, QC, c.NOWN], BF16, "qnT")
        cq_s = sb([64, 2, c.NOWN], F32, "cq")
        wq_r = Ring("wq", [sb([128, QC, 256], BF16, "wq") for _ in range(2)])
        sqb_r = Ring("sqb", [sb([128, 512], BF16, "sqb") for _ in range(2)])
        sqr_r = Ring("sqr", [sb([64, 512], BF16, "sqr") for _ in range(2)])
        lnv = sb([128, 512], F32, "lnv")
        rstd_r = Ring("rstd", [sb([128, 512], F32, "rstd") for _ in range(2)])
        rt_t = sb([64, 2, 512], F32, "rt")
        rsum = sb([64, 512], F32, "rsum")
        qtmp = sb([128, 512], F32, "qtmp")
        qn_r = Ring("qn", [sb([128, 512], BF16, "qn") for _ in range(3)])
        qr_r = Ring("qr", [sb([64, 512], BF16, "qr") for _ in range(3)])
        ld(qnT[:, :, :], qnT_d.ap().rearrange("c p n -> p c n"), "qnT")
        zt = sb([128, D], BF16, "zt")
        P.op("dve", lambda e: e.memset(zt[:, :], 0.0), writes=["zt"])
        for ex in range(NE):
            st(Xe[ex * 128:(ex + 1) * 128, :], zt[:, :], "zt")
        ld(cq_s[:, 0, :], cosq[:, :], "cq")
        ld(cq_s[:, 1, :], sinq[:, :], "cq")
        wqn_v = wqn.ap().rearrange("(c p) n -> p c n", p=128)
        wqr_v = wqr.ap().rearrange("(c p) n -> p c n", p=128)
        wqs_v = wqs.ap().rearrange("(c p) n -> p c n", p=128)
        for h in range(H):
            wqname, wq = wq_r.next()
            ld_cast(wq[:, :, 0:128], wqn_v[:, :, h * 128:(h + 1) * 128], wqname)
            ld_cast(wq[:, :, 128:192], wqr_v[:, :, h * 64:(h + 1) * 64], wqname)
            ld_cast(wq[:, :, 192:256], wqs_v[:, :, h * 64:(h + 1) * 64], wqname)
            for s in range(c.NSLOT):
                t0 = s * 512
                bn = 2 + (s % 2)

                def f(e, wq=wq, t0=t0, bn=bn):
                    ins = None
                    for k in range(QC):
                        ins = e.matmul(ps[bn][:, :], lhsT=wq[:, k, 0:128], rhs=qnT[:, k, t0:t0 + 512], start=(k == 0), stop=(k == QC - 1))
                    for v in range(2):
                        for k in range(QC):
                            ins = e.matmul(ps[5 + v][:64, :], lhsT=wq[:, k, 128 + 64 * v:192 + 64 * v], rhs=qnT[:, k, t0:t0 + 512],
                                           start=(k == 0), stop=(k == QC - 1))
                    return ins
                P.op("pe", f, reads=["qnT", wqname], writes=["ps%d" % bn, "ps5", "ps6"])
                sqn, sq = sqb_r.next()
                P.op("act", lambda e, sq=sq, bn=bn: e.activation(out=sq[:, :], in_=ps[bn][:, :], func=AF.Square),
                     reads=["ps%d" % bn], writes=[sqn])
                srn, sr = sqr_r.next()
                P.op("act", lambda e, sr=sr: e.activation(out=sr[:, :], in_=ps[5][:64, :], func=AF.Square),
                     reads=["ps5"], writes=[srn])

                def f2(e, sq=sq, sr=sr):
                    e.matmul(ps[4][:, :], lhsT=ones[:, :], rhs=sq[:, :], start=True, stop=False)
                    return e.matmul(ps[4][:, :], lhsT=ones[:64, :], rhs=sr[:, :], start=False, stop=True)
                P.op("pe", f2, reads=[sqn, srn, "ones"], writes=["ps4"])
                rsn, rs = rstd_r.next()
                rstd_from_psum(4, 192, 512, "lnv", lnv, rsn, rs)
                qnn, qn = qn_r.next()
                P.op("act", lambda e, bn=bn: e.activation(out=qtmp[:, :], in_=ps[bn][:, :], func=AF.Copy, scale=gq_s[:, 0:1]),
                     reads=["ps%d" % bn, "gq"], writes=["qtmp"])
                P.op("dve", lambda e, qn=qn, rs=rs: e.tensor_tensor(out=qn[:, :], in0=qtmp[:, :], in1=rs[:, :], op=ALU.mult),
                     reads=["qtmp", rsn], writes=[qnn])
                for v in range(2):
                    P.op("act", lambda e, v=v: e.activation(out=rt_t[:, v, :], in_=ps[5 + v][:64, :], func=AF.Copy,
                                                            scale=gq_s[:64, 1 + v:2 + v]),
                         reads=["ps%d" % (5 + v), "gq"], writes=["rt%d" % v])
                    P.op("dve", lambda e, v=v, t0=t0: e.tensor_tensor(out=rt_t[:, v, :], in0=rt_t[:, v, :], in1=cq_s[:, v, t0:t0 + 512], op=ALU.mult),
                         reads=["rt%d" % v, "cq"], writes=["rt%d" % v])
                P.op("dve", lambda e: e.tensor_tensor(out=rsum[:, :], in0=rt_t[:, 0, :], in1=rt_t[:, 1, :], op=ALU.add),
                     reads=["rt0", "rt1"], writes=["rsum"])
                qrn, qr = qr_r.next()
                P.op("dve", lambda e, qr=qr, rs=rs: e.tensor_tensor(out=qr[:, :], in0=rsum[:, :], in1=rs[:64, :], op=ALU.mult),
                     reads=["rsum", rsn], writes=[qrn])
                st(QTn[h, :, t0:t0 + 512], qn[:, :], qnn)
                st(QTr[h, :, t0:t0 + 512], qr[:, :], qrn)
        P.barrier()

    if STOP is None or 2 <= STOP:
        phase_2()

    def phase_3():
        nonlocal_dummy = None
        state["off"] = PERSIST
        mask_s = sb([128, 16, 512], BF16, "mask")
        ld_cast(mask_s[:, :, :], maskc[:, :, :], "mask")
        ktn_r = Ring("ktn", [sb([128, LKP], BF16, "ktn") for _ in range(2)])
        ktr_r = Ring("ktr", [sb([64, LKP], BF16, "ktr") for _ in range(2)])
        v_r = Ring("v", [sb([128, NKB, 128], BF16, "v") for _ in range(2)])
        qn_r = Ring("qn", [sb([128, c.NOWN], BF16, "qn") for _ in range(2)])
        qr_r = Ring("qr", [sb([64, c.NOWN], BF16, "qr") for _ in range(2)])
        p_r = Ring("p", [sb([128, 512], BF16, "p") for _ in range(4)])
        rl = sb([128, 512], F32, "rl")
        y_r = Ring("y", [sb([128, 512], BF16, "y") for _ in range(2)])
        S_BANKS = [0, 1, 2, 3]
        sctr = [0]
        oidx = 0
        for h in range(H):
            ktnn, ktn = ktn_r.next()
            ktrn, ktr = ktr_r.next()
            vn, vv = v_r.next()
            qnn, qn = qn_r.next()
            qrn, qr = qr_r.next()
            LK = c.SEQ + c.NMETA
            ld(ktn[:, :LK], KTn[h, :, :LK], ktnn)
            ld(ktr[:, :LK], KTr[h, :, :LK], ktrn)
            ld(vv[:, 0:NKB - 1, :], Vd[h, :, 0:NKB - 1, :], vn)
            ld(vv[:c.NMETA, NKB - 1, :], Vd[h, :c.NMETA, NKB - 1, :], vn)
            ld(qn[:, :], QTn[h, :, :], qnn)
            ld(qr[:, :], QTr[h, :, :], qrn)
            for s in range(c.NSLOT):
                t0 = s * 512
                kbs = [(NKB - 1, c.NMETA, None)] + [(kb, 128, (kb - 16 * s) if kb >= 16 * s else None) for kb in range(16 * (s + 1))]
                ob = 4 + (oidx % 2)
                lb = 6 + (oidx % 2)
                oidx += 1
                pend = None
                nkb = len(kbs)

                def emit_s(i):
                    kb, nk, mk = kbs[i]
                    bank = S_BANKS[sctr[0] % 4]
                    sctr[0] += 1

                    def f(e, kb=kb, nk=nk, bank=bank, ktn=ktn, ktr=ktr, qn=qn, qr=qr, t0=t0):
                        e.matmul(ps[bank][:nk, :], lhsT=ktn[:, kb * 128:kb * 128 + nk], rhs=qn[:, t0:t0 + 512], start=True, stop=False)
                        return e.matmul(ps[bank][:nk, :], lhsT=ktr[:, kb * 128:kb * 128 + nk], rhs=qr[:, t0:t0 + 512], start=False, stop=True)
                    P.op("pe", f, reads=[ktnn, ktrn, qnn, qrn], writes=["ps%d" % bank])
                    pn, pt = p_r.next()
                    P.op("act", lambda e, nk=nk, bank=bank, pt=pt: e.activation(out=pt[:nk, :], in_=ps[bank][:nk, :], func=AF.Exp),
                         reads=["ps%d" % bank], writes=[pn])
                    if mk is not None:
                        P.op("dve", lambda e, pt=pt, mk=mk: e.tensor_tensor(out=pt[:, :], in0=pt[:, :], in1=mask_s[:, mk, :], op=ALU.mult),
                             reads=[pn, "mask"], writes=[pn])
                    return (pn, pt, kb, nk)

                def emit_pv(i, item):
                    pn, pt, kb, nk = item

                    def f(e, pt=pt, kb=kb, nk=nk, i=i, vv=vv, ob=ob, lb=lb, nkb=nkb):
                        e.matmul(ps[ob][:, :], lhsT=vv[:nk, kb, :], rhs=pt[:nk, :], start=(i == 0), stop=(i == nkb - 1))
                        return e.matmul(ps[lb][:, :], lhsT=ones[:nk, :], rhs=pt[:nk, :], start=(i == 0), stop=(i == nkb - 1))
                    P.op("pe", f, reads=[pn, vn, "ones"], writes=["ps%d" % ob, "ps%d" % lb])

                items = {}
                items[0] = emit_s(0)
                for i in range(nkb):
                    if i + 1 < nkb:
                        items[i + 1] = emit_s(i + 1)
                    emit_pv(i, items.pop(i))
                P.op("dve", lambda e, lb=lb: e.reciprocal(out=rl[:, :], in_=ps[lb][:, :]), reads=["ps%d" % lb], writes=["rl"])
                yn, y = y_r.next()
                P.op("dve", lambda e, y=y, ob=ob: e.tensor_tensor(out=y[:, :], in0=ps[ob][:, :], in1=rl[:, :], op=ALU.mult),
                     reads=["ps%d" % ob, "rl"], writes=[yn])
                st(mixT[PC + h, :, t0:t0 + 512], y[:, :], yn)
        P.barrier()

    if STOP is None or 3 <= STOP:
        phase_3()

    def phase_4():
        nonlocal_dummy = None
        state["off"] = PERSIST
        mt = sb([128, DC, 512], BF16, "mt")
        wo_r = Ring("wo", [sb([128, DC, 512], BF16, "wo") for _ in range(2)])
        xr_r = Ring("xr", [sb([128, 512], F32, "xr") for _ in range(4)])
        hr_r = Ring("hr", [sb([128, 512], F32, "hr") for _ in range(4)])
        w_out_v = w_out.ap().rearrange("(c p) n -> p c n", p=128)
        for s in range(c.NSLOT):
            ld(mt[:, :, :], mixT[:, :, s * 512:(s + 1) * 512].rearrange("c p n -> p c n"), "mt")
            for ct in range(c.CT):
                won, wo = wo_r.next()
                ld_cast(wo[:, :, :], w_out_v[:, :, ct * 512:(ct + 1) * 512], won)
                for blk in range(4):
                    bank = (ct * 4 + blk) % 8
                    xrn, xr = xr_r.next()
                    r0 = s * 528 + blk * 128
                    ld(xr[:, :], xo[r0:r0 + 128, ct * 512:(ct + 1) * 512], xrn)

                    def f(e, wo=wo, blk=blk, bank=bank):
                        ins = None
                        for k in range(DC):
                            ins = e.matmul(ps[bank][:, :], lhsT=mt[:, k, blk * 128:(blk + 1) * 128], rhs=wo[:, k, :],
                                           start=(k == 0), stop=(k == DC - 1))
                        return ins
                    P.op("pe", f, reads=["mt", won], writes=["ps%d" % bank])
                    hrn, hr = hr_r.next()
                    P.op("dve", lambda e, hr=hr, xr=xr, bank=bank: e.tensor_tensor(out=hr[:, :], in0=ps[bank][:, :], in1=xr[:, :], op=ALU.add),
                         reads=["ps%d" % bank, xrn], writes=[hrn])
                    o0 = s * 512 + blk * 128
                    st(out[o0:o0 + 128, ct * 512:(ct + 1) * 512], hr[:, :], hrn)
        P.barrier()

    if STOP is None or 4 <= STOP:
        phase_4()

    def phase_5():
        nonlocal_dummy = None
        state["off"] = PERSIST
        gbc = sb([128, D], F32, "gbc")
        ht_r = Ring("ht", [sb([128, D], F32, "ht") for _ in range(2)])
        hn_r = Ring("hn", [sb([128, D], BF16, "hn") for _ in range(2)])
        junk = sb([128, D], BF16, "junk")
        hnT = sb([128, DC, 128], BF16, "hnT")
        stat_r = Ring("stat", [sb([128, 4], F32, "stat") for _ in range(2)])
        Wr = sb([128, DC, c.NRC], BF16, "Wr")
        br_s = sb([128, c.NRC], F32, "br")
        tri = sb([128, 128], BF16, "tri")
        iote = sb([128, NE], F32, "iote")
        lg = sb([128, c.NRC], F32, "lg")
        rt_ = sb([128, 16], F32, "rtmp")
        exg = sb([128, 8], F32, "exg")
        ohg = sb([128, 8], F32, "ohg")
        pen = sb([128, 8], F32, "pen")
        msk = sb([128, NE], F32, "msk")
        oh1 = sb([128, NE], F32, "oh1")
        oh2 = sb([128, NE], F32, "oh2")
        ohb = sb([128, NE], BF16, "ohb")
        srun = sb([128, NE], F32, "srun")
        srun_b = sb([128, NE], BF16, "srunb")
        posf = sb([128, NE], F32, "posf")
        tmpe = sb([128, NE], F32, "tmpe")
        destf = sb([128, 2], F32, "destf")
        ld(gbc[:, :], gffn_bc[:, :], "gbc")
        ld_cast(Wr[:, :, :], w_r.ap().rearrange("(c p) n -> p c n", p=128), "Wr")
        ld(br_s[:, :], b_r[:, :], "br")
        ld_cast(tri[:, :], tri_d[:, :], "tri")
        ld(iote[:, :], iota_d[:, :], "iote")
        P.op("dve", lambda e: e.memset(srun[:, :], 0.0), writes=["srun"])
        EPG = c.EPG
        for tb in range(4 * c.NSLOT):
            htn, ht = ht_r.next()
            ld(ht[:, :], out[tb * 128:(tb + 1) * 128, :], htn)
            stn, stt_ = stat_r.next()
            hnn, hn = hn_r.next()
            rms_rows(htn, ht, 128, "junk", junk, gbc, hnn, hn, stt_, stn)
            transpose_block(hnn, hn, 128, DC, "hnT", hnT, 0, banks=(0, 1))

            def f(e):
                ins = None
                for k in range(DC):
                    ins = e.matmul(ps[2][:, 0:c.NRC], lhsT=hnT[:, k, :], rhs=Wr[:, k, :], start=(k == 0), stop=(k == DC - 1))
                return ins
            P.op("pe", f, reads=["hnT", "Wr"], writes=["ps2"])
            R = "route"
            P.op("dve", lambda e: e.tensor_tensor(out=lg[:, :], in0=ps[2][:, 0:c.NRC], in1=br_s[:, :], op=ALU.add),
                 reads=["ps2", "br"], writes=[R])
            P.op("dve", lambda e: e.tensor_reduce(out=rt_[:, 0:1], in_=lg[:, 0:8], axis=AX.X, op=ALU.max), reads=[R], writes=[R])
            P.op("dve", lambda e: e.tensor_scalar(out=rt_[:, 1:2], in0=rt_[:, 0:1], scalar1=-1.0, scalar2=None, op0=ALU.mult), reads=[R], writes=[R])
            P.op("act", lambda e: e.activation(out=exg[:, :], in_=lg[:, 0:8], func=AF.Exp, bias=rt_[:, 1:2], accum_out=rt_[:, 2:3]),
                 reads=[R], writes=[R])
            P.op("dve", lambda e: e.reciprocal(out=rt_[:, 3:4], in_=rt_[:, 2:3]), reads=[R], writes=[R])
            P.op("dve", lambda e: e.tensor_scalar(out=ohg[:, :], in0=lg[:, 0:8], scalar1=rt_[:, 0:1], scalar2=None, op0=ALU.is_equal),
                 reads=[R], writes=[R])
            P.op("dve", lambda e: e.tensor_scalar(out=pen[:, :], in0=ohg[:, :], scalar1=1e30, scalar2=-1e30, op0=ALU.mult, op1=ALU.add),
                 reads=[R], writes=[R])
            for g_ in range(8):
                P.op("dve", lambda e, g_=g_: e.tensor_scalar(out=msk[:, g_ * EPG:(g_ + 1) * EPG], in0=lg[:, 8 + g_ * EPG:8 + (g_ + 1) * EPG],
                                                            scalar1=pen[:, g_:g_ + 1], scalar2=None, op0=ALU.add), reads=[R], writes=[R])
            P.op("dve", lambda e: e.tensor_reduce(out=rt_[:, 4:5], in_=msk[:, :], axis=AX.X, op=ALU.max), reads=[R], writes=[R])
            P.op("dve", lambda e: e.tensor_scalar(out=oh1[:, :], in0=msk[:, :], scalar1=rt_[:, 4:5], scalar2=None, op0=ALU.is_equal),
                 reads=[R], writes=[R])
            P.op("dve", lambda e: e.scalar_tensor_tensor(out=msk[:, :], in0=oh1[:, :], scalar=-1e30, in1=msk[:, :], op0=ALU.mult, op1=ALU.add),
                 reads=[R], writes=[R])
            P.op("dve", lambda e: e.tensor_reduce(out=rt_[:, 5:6], in_=msk[:, :], axis=AX.X, op=ALU.max), reads=[R], writes=[R])
            P.op("dve", lambda e: e.tensor_scalar(out=oh2[:, :], in0=msk[:, :], scalar1=rt_[:, 5:6], scalar2=None, op0=ALU.is_equal),
                 reads=[R], writes=[R])
            P.op("dve", lambda e: e.tensor_tensor(out=rt_[:, 6:7], in0=rt_[:, 5:6], in1=rt_[:, 4:5], op=ALU.subtract), reads=[R], writes=[R])
            P.op("act", lambda e: e.activation(out=rt_[:, 7:8], in_=rt_[:, 6:7], func=AF.Exp), reads=[R], writes=[R])
            P.op("dve", lambda e: e.tensor_scalar(out=rt_[:, 7:8], in0=rt_[:, 7:8], scalar1=1.0, scalar2=None, op0=ALU.add), reads=[R], writes=[R])
            P.op("dve", lambda e: e.reciprocal(out=rt_[:, 8:9], in_=rt_[:, 7:8]), reads=[R], writes=[R])
            P.op("dve", lambda e, tb=tb: e.tensor_tensor(out=wgt_all[:, tb, 0:1], in0=rt_[:, 8:9], in1=rt_[:, 3:4], op=ALU.mult),
                 reads=[R], writes=[R, "wgt"])
            P.op("dve", lambda e, tb=tb: e.tensor_tensor(out=wgt_all[:, tb, 1:2], in0=rt_[:, 3:4], in1=wgt_all[:, tb, 0:1], op=ALU.subtract),
                 reads=[R, "wgt"], writes=[R, "wgt"])
            P.op("dve", lambda e: e.tensor_tensor(out=ohb[:, :], in0=oh1[:, :], in1=oh2[:, :], op=ALU.add), reads=[R], writes=["ohb"])
            P.op("dve", lambda e: e.tensor_copy(out=srun_b[:, :], in_=srun[:, :]), reads=["srun"], writes=["srunb"])

            def f3(e):
                e.matmul(ps[3][:, 0:NE], lhsT=tri[:, :], rhs=ohb[:, :], start=True, stop=False)
                return e.matmul(ps[3][:, 0:NE], lhsT=ones[:, :], rhs=srun_b[:, :], start=False, stop=True)
            P.op("pe", f3, reads=["ohb", "srunb", "tri", "ones"], writes=["ps3"])
            P.op("dve", lambda e: e.tensor_tensor(out=srun[:, :], in0=srun[:, :], in1=ohb[:, :], op=ALU.add), reads=["ohb", "srun"], writes=["srun"])
            P.op("dve", lambda e: e.tensor_tensor(out=posf[:, :], in0=ps[3][:, 0:NE], in1=iote[:, :], op=ALU.add), reads=["ps3", "iote"], writes=[R])
            for k_, ohk in enumerate((oh1, oh2)):
                P.op("dve", lambda e, ohk=ohk: e.tensor_tensor(out=tmpe[:, :], in0=posf[:, :], in1=ohk[:, :], op=ALU.mult), reads=[R], writes=[R])
                P.op("dve", lambda e, k_=k_: e.tensor_reduce(out=destf[:, k_:k_ + 1], in_=tmpe[:, :], axis=AX.X, op=ALU.add), reads=[R], writes=[R])
            P.op("dve", lambda e, tb=tb: e.tensor_copy(out=dest_all[:, tb, :], in_=destf[:, :]), reads=[R], writes=["dest%d" % tb, R])
            for k_ in range(2):
                P.dma("pool", lambda e, tb=tb, k_=k_, hn=hn: e.indirect_dma_start(
                    out=Xe[:, :], out_offset=bass.IndirectOffsetOnAxis(ap=dest_all[:, tb, k_:k_ + 1], axis=0),
                    in_=hn[:, :], in_offset=None), reads=[hnn, "dest%d" % tb])
        P.barrier()

    if STOP is None or 5 <= STOP:
        phase_5()

    def phase_6():
        nonlocal_dummy = None
        state["off"] = PERSIST
        xe_r = Ring("xe", [sb([128, D], BF16, "xe") for _ in range(2)])
        xeT = sb([128, DC, 128], BF16, "xeT")
        KG = 8
        NKG = DC // KG
        wg_r = Ring("wg", [sb([128, KG, DE], BF16, "wg") for _ in range(3)])
        wu_r = Ring("wu", [sb([128, KG, DE], BF16, "wu") for _ in range(3)])
        wd_r = Ring("wd", [sb([128, c.EC, 512], BF16, "wd") for _ in range(3)])
        sg = sb([128, DE], F32, "sg")
        hb = sb([128, DE], BF16, "hb")
        hbT = sb([128, c.EC, 128], BF16, "hbT")
        yo_r = Ring("yo", [sb([128, 512], F32, "yo") for _ in range(4)])
        for ex in range(NE):
            xen, xe = xe_r.next()
            ld(xe[:, :], Xe[ex * 128:(ex + 1) * 128, :], xen)
            transpose_block(xen, xe, 128, DC, "xeT", xeT, 0, banks=(0, 1))
            wgv = w_gate[ex, :, :].rearrange("(c p) n -> p c n", p=128)
            wuv = w_up[ex, :, :].rearrange("(c p) n -> p c n", p=128)
            for kg in range(NKG):
                wgn, wg_ = wg_r.next()
                wun, wu_ = wu_r.next()
                ld_cast(wg_[:, :, :], wgv[:, kg * KG:(kg + 1) * KG, :], wgn)
                ld_cast(wu_[:, :, :], wuv[:, kg * KG:(kg + 1) * KG, :], wun)

                def f(e, kg=kg, wg_=wg_, wu_=wu_):
                    ins = None
                    for k in range(KG):
                        kk = kg * KG + k
                        e.matmul(ps[2][:, 0:DE], lhsT=xeT[:, kk, :], rhs=wg_[:, k, :], start=(kk == 0), stop=(kk == DC - 1))
                        ins = e.matmul(ps[3][:, 0:DE], lhsT=xeT[:, kk, :], rhs=wu_[:, k, :], start=(kk == 0), stop=(kk == DC - 1))
                    return ins
                P.op("pe", f, reads=["xeT", wgn, wun], writes=["ps2", "ps3"])
            P.op("act", lambda e: e.activation(out=sg[:, :], in_=ps[2][:, 0:DE], func=AF.Silu), reads=["ps2"], writes=["sg"])
            P.op("dve", lambda e: e.tensor_tensor(out=hb[:, :], in0=sg[:, :], in1=ps[3][:, 0:DE], op=ALU.mult), reads=["sg", "ps3"], writes=["hb"])
            transpose_block("hb", hb, 128, c.EC, "hbT", hbT, 0, banks=(0, 1))
            for ct in range(c.CT):
                wdn, wd_ = wd_r.next()
                ld_cast(wd_[:, :, :], w_down[ex, :, ct * 512:(ct + 1) * 512].rearrange("(c p) n -> p c n", p=128), wdn)
                bank = 4 + (ct % 4)

                def f(e, wd_=wd_, bank=bank):
                    ins = None
                    for k in range(c.EC):
                        ins = e.matmul(ps[bank][:, :], lhsT=hbT[:, k, :], rhs=wd_[:, k, :], start=(k == 0), stop=(k == c.EC - 1))
                    return ins
                P.op("pe", f, reads=["hbT", wdn], writes=["ps%d" % bank])
                yon, yo = yo_r.next()
                if ct % 2 == 0:
                    P.op("act", lambda e, yo=yo, bank=bank: e.activation(out=yo[:, :], in_=ps[bank][:, :], func=AF.Copy),
                         reads=["ps%d" % bank], writes=[yon])
                else:
                    P.op("dve", lambda e, yo=yo, bank=bank: e.tensor_copy(out=yo[:, :], in_=ps[bank][:, :]),
                         reads=["ps%d" % bank], writes=[yon])
                st(Ye[ex * 128:(ex + 1) * 128, ct * 512:(ct + 1) * 512], yo[:, :], yon)
        P.barrier()

    if STOP is None or 6 <= STOP:
        phase_6()

    def phase_7():
        nonlocal_dummy = None
        state["off"] = PERSIST
        ht_r = Ring("ht", [sb([128, D], F32, "ht") for _ in range(2)])
        y0_r = Ring("y0", [sb([128, D], F32, "y0") for _ in range(2)])
        y1_r = Ring("y1", [sb([128, D], F32, "y1") for _ in range(2)])
        for tb in range(4 * c.NSLOT):
            htn, ht = ht_r.next()
            ld(ht[:, :], out[tb * 128:(tb + 1) * 128, :], htn)
            y0n, y0 = y0_r.next()
            y1n, y1 = y1_r.next()
            for k_, (yn_, yy) in enumerate(((y0n, y0), (y1n, y1))):
                P.dma("pool", lambda e, tb=tb, k_=k_, yy=yy: e.indirect_dma_start(
                    out=yy[:, :], out_offset=None, in_=Ye[:, :],
                    in_offset=bass.IndirectOffsetOnAxis(ap=dest_all[:, tb, k_:k_ + 1], axis=0)), writes=[yn_])
            P.op("dve", lambda e, tb=tb, ht=ht, y0=y0: e.scalar_tensor_tensor(out=ht[:, :], in0=y0[:, :], scalar=wgt_all[:, tb, 0:1], in1=ht[:, :],
                                                                             op0=ALU.mult, op1=ALU.add), reads=[y0n, htn], writes=[htn])
            P.op("dve", lambda e, tb=tb, ht=ht, y1=y1: e.scalar_tensor_tensor(out=ht[:, :], in0=y1[:, :], scalar=wgt_all[:, tb, 1:2], in1=ht[:, :],
                                                                             op0=ALU.mult, op1=ALU.add), reads=[y1n, htn], writes=[htn])
            st(out[tb * 128:(tb + 1) * 128, :], ht[:, :], htn)
        P.barrier()

    if STOP is None or 7 <= STOP:
        phase_7()

    import os as _os
    if "KMAXOPS" in _os.environ:
        n = int(_os.environ["KMAXOPS"])
        P.ops = P.ops[:n]
        P.last = {e: None for e in ENGS}
        P.dma_last = {}
        for i, o in enumerate(P.ops):
            P.last[o["eng"]] = i
            if o["kind"] == "d":
                P.dma_last[o["semkey"]] = i
        P.barrier()
    P.emit(nc, es)
    es.close()
    return nc


def host_layout(cfg, inputs):
    c = cfg
    f32 = np.float32
    g = {k: np.asarray(v) for k, v in inputs.items()}
    x = g["x"]
    D = c.D
    PW, QL, KL, H = c.PW, c.QL, c.KL, c.H
    w_in = g["w_in"][0]
    u_c = w_in[:, :PW]
    q_c = w_in[:, PW:PW + QL]
    kv_c = w_in[:, PW + QL:PW + QL + KL]
    r_c = w_in[:, PW + QL + KL:]
    r_sw = np.concatenate([r_c[:, 32:], r_c[:, :32]], axis=1)
    w_kv = np.ascontiguousarray(np.concatenate([kv_c, r_c, r_sw], axis=1))
    w_own = np.ascontiguousarray(np.concatenate([u_c, q_c], axis=1))
    w_ukv = g["w_ukv"][0].reshape(KL, H, 256)
    wk = np.ascontiguousarray(w_ukv[:, :, :128].reshape(KL, H * 128))
    wv = np.ascontiguousarray(w_ukv[:, :, 128:].reshape(KL, H * 128))
    w_uq = g["w_uq"][0].reshape(QL, H, 192)
    wqn = np.ascontiguousarray(w_uq[:, :, :128].reshape(QL, H * 128))
    wqr = np.ascontiguousarray(w_uq[:, :, 128:].reshape(QL, H * 64))
    wqs = np.ascontiguousarray(np.concatenate([w_uq[:, :, 160:], w_uq[:, :, 128:160]], axis=2).reshape(QL, H * 64))

    def headg(v):
        o = np.zeros((128, 3), f32)
        o[:, 0] = v[:128]
        o[:64, 1] = v[128:]
        o[:64, 2] = np.concatenate([v[160:], v[128:160]])
        return o

    def pc(v):
        return np.ascontiguousarray(v.reshape(-1, 128).T)

    inv = (1.0 / (10000.0 ** (np.arange(0, 64, 2, dtype=f32) / f32(64)))).astype(f32)

    def tables(pos):
        ang = pos.astype(f32)[:, None] * inv[None, :]
        co, si = np.cos(ang).astype(f32), np.sin(ang).astype(f32)
        return (np.ascontiguousarray(np.concatenate([co, co], 1).T), np.ascontiguousarray(np.concatenate([-si, si], 1).T))

    posk = np.zeros(c.LKP, np.int64)
    posk[:c.SEQ] = c.NMETA + np.arange(c.SEQ)
    posk[c.SEQ:c.SEQ + c.NMETA] = np.arange(c.NMETA)
    cosk, sink = tables(posk)
    shared = dict(
        meta=np.ascontiguousarray(g["meta_tokens"].astype(f32)),
        cosk=cosk, sink=sink,
        ident=np.eye(128, dtype=f32), ones=np.ones((128, 128), f32),
        tri=np.triu(np.ones((128, 128), f32), 1),
        iotae=np.ascontiguousarray(np.broadcast_to((np.arange(c.NE, dtype=f32) * 128)[None, :], (128, c.NE))),
        gmix_bc=np.ascontiguousarray(np.broadcast_to(g["mix_norm_g"][0][None, :], (128, D))),
        gffn_bc=np.ascontiguousarray(np.broadcast_to(g["ffn_norm_g"][0][None, :], (128, D))),
        w_kv=w_kv, w_own=w_own, gkv=pc(g["kv_lat_norm_g"][0]), gql=pc(g["q_lat_norm_g"][0]),
        wk=wk, wv=wv, wqn=wqn, wqr=wqr, wqs=wqs,
        gk=headg(g["k_head_norm_g"][0]), gq=headg(g["q_head_norm_g"][0]),
        wpool=np.ascontiguousarray(g["w_pool"][0]), pscale=pc(g["pool_scale"][0]),
        w_out=np.ascontiguousarray(g["w_out"][0]),
        w_r=np.ascontiguousarray(np.concatenate([g["w_group"][0], g["w_expert"][0]], axis=1)),
        b_r=np.ascontiguousarray(np.broadcast_to(np.concatenate([g["b_group"][0], g["b_expert"][0]])[None, :], (128, c.NRC))),
        w_gate=g["w_gate"][0], w_up=g["w_up"][0], w_down=g["w_down"][0],
    )
    maps = []
    for core in range(8):
        b, j = core // 4, core % 4
        xo = np.empty((c.NSLOT * 528, D), f32)
        posq = np.empty(c.NOWN, np.int64)
        for s in range(c.NSLOT):
            t0 = (4 * s + j) * 512
            xo[s * 528:s * 528 + 512] = x[b, t0:t0 + 512]
            xo[s * 528 + 512:(s + 1) * 528] = x[b, t0 - 16:t0] if t0 > 0 else shared["meta"]
            posq[s * 512:(s + 1) * 512] = c.NMETA + t0 + np.arange(512)
        cosq, sinq = tables(posq)
        mk = np.zeros((128, 16, 512), f32)
        p = np.arange(128)[:, None]
        cc = np.arange(128)[None, :]
        for kbw in range(16):
            for qb in range(4):
                qpos = 4 * j + qb
                if kbw < qpos:
                    mk[:, kbw, qb * 128:(qb + 1) * 128] = 1.0
                elif kbw == qpos:
                    mk[:, kbw, qb * 128:(qb + 1) * 128] = (p <= cc).astype(f32)
        m = dict(shared)
        m.update(xb=np.ascontiguousarray(x[b]), xo=xo, cosq=cosq, sinq=sinq, maskc=mk)
        maps.append(m)
    return maps


_CACHE = {}


def kernel(**inputs):
    cfg = Cfg()
    if "nc" not in _CACHE:
        _CACHE["nc"] = build(cfg)
    nc = _CACHE["nc"]
    maps = host_layout(cfg, inputs)
    res = run_bass_kernel_spmd(nc, maps, core_ids=list(range(8)))
    outp = np.empty((2, cfg.SEQ, cfg.D), np.float32)
    for core in range(8):
        b, j = core // 4, core % 4
        o = np.asarray(res.results[core]["out"])
        for s in range(cfg.NSLOT):
            t0 = (4 * s + j) * 512
            outp[b, t0:t0 + 512] = o[s * 512:(s + 1) * 512]
    return outp
```
